# Optimizing a Trainium2 kernel written in Bass

```python
import jax, jax.numpy as jnp
from jax import lax
import numpy as np

D_MODEL = 1024
BATCH = 8
SEQ = 8192
DEPTH = 2

HEAD_DIM = 64
N_HEADS_DIL = 8
N_HEADS_NA = 8
D_DIL = N_HEADS_DIL * HEAD_DIM
D_NA = N_HEADS_NA * HEAD_DIM
D_MIX = D_DIL + D_NA
ATTN_SCALE = HEAD_DIM ** -0.5
ROPE_THETA = 10000.0
DIL_PATTERNS = ((128, 1), (512, 4), (2048, 16))
DIL_BLOCK = 64
GRID_W = 64
NA_ROWS_MAX = 8
NA_COLS = 16
NA_QCOLS = 16
NA_KCOLS = NA_QCOLS + NA_COLS
N_EXPERTS = 16
EXPERT_FF = 2816
EC_CAPACITY = 2
EPS = 1e-6
NEG_INF = -1e30

kernel_name = "hybrid_dilated_neighbourhood_ec_moe_encoder"


def _rmsnorm(x, g):
    xf = x.astype(jnp.float32)
    y = xf * lax.rsqrt(jnp.mean(xf * xf, axis=-1, keepdims=True) + EPS)
    return (y * g.astype(jnp.float32)).astype(x.dtype)


def _rope_tables(T):
    pos = jnp.arange(T, dtype=jnp.float32)
    inv = ROPE_THETA ** (-jnp.arange(0, HEAD_DIM, 2, dtype=jnp.float32) / HEAD_DIM)
    ang = pos[:, None] * inv[None, :]
    return jnp.cos(ang), jnp.sin(ang)


def _apply_rope(x, cos, sin):
    xf = x.astype(jnp.float32)
    x1, x2 = jnp.split(xf, 2, axis=-1)
    return jnp.concatenate([x1 * cos - x2 * sin, x2 * cos + x1 * sin], axis=-1).astype(x.dtype)


def _heads(a, n):
    B, T, _ = a.shape
    return a.reshape(B, T, n, HEAD_DIM).transpose(0, 2, 1, 3)


def _unheads(a):
    B, H, T, hd = a.shape
    return a.transpose(0, 2, 1, 3).reshape(B, T, H * hd)


def _dilated_branch(q, k, v, window, dilation):
    B, H, T, hd = q.shape
    half = window // (2 * dilation)
    L = T // dilation
    blk = DIL_BLOCK
    nb = -(-L // blk)
    nbr = -(-half // blk)
    span = (2 * nbr + 1) * blk
    Lp = nb * blk

    def strided(a):
        return a.reshape(B, H, L, dilation, hd).transpose(0, 1, 3, 2, 4)

    qs = jnp.pad(strided(q), ((0, 0), (0, 0), (0, 0), (0, Lp - L), (0, 0)))
    qs = qs.reshape(B, H, dilation, nb, blk, hd)
    pad_k = ((0, 0), (0, 0), (0, 0), (nbr * blk, Lp - L + nbr * blk), (0, 0))

    def windows(a):
        ab = jnp.pad(strided(a), pad_k).reshape(B, H, dilation, nb + 2 * nbr, blk, hd)
        return jnp.concatenate([ab[:, :, :, sh:sh + nb] for sh in range(2 * nbr + 1)], axis=4)

    ks, vs = windows(k), windows(v)
    q_idx = jnp.arange(nb)[:, None] * blk + jnp.arange(blk)[None, :]
    k_idx = jnp.arange(nb)[:, None] * blk + jnp.arange(span)[None, :] - nbr * blk
    rel = k_idx[:, None, :] - q_idx[:, :, None]
    valid = (jnp.abs(rel) <= half) & (k_idx[:, None, :] >= 0) & (k_idx[:, None, :] < L)

    s = jnp.einsum('bhrnqd,bhrnkd->bhrnqk', qs, ks, preferred_element_type=jnp.float32) * ATTN_SCALE
    s = jnp.where(valid, s, NEG_INF)
    m = jnp.max(s, axis=-1, keepdims=True)
    p = jnp.exp(s - m)
    den = jnp.sum(p, axis=-1, keepdims=True)
    o = jnp.einsum('bhrnqk,bhrnkd->bhrnqd', p.astype(v.dtype), vs,
                   preferred_element_type=jnp.float32) / den
    lse = (m + jnp.log(den))[..., 0]

    def unstrided(a):
        a = a.reshape(B, H, dilation, Lp, *a.shape[5:])[:, :, :, :L]
        return jnp.moveaxis(a, 2, 3).reshape(B, H, T, *a.shape[4:])

    return unstrided(o), unstrided(lse)


def _dilated_attention(q, k, v):
    outs, lses = zip(*[_dilated_branch(q, k, v, w, d) for (w, d) in DIL_PATTERNS])
    wts = jax.nn.softmax(jnp.stack(lses), axis=0)
    return jnp.sum(wts[..., None] * jnp.stack(outs), axis=0)


def _neighbourhood_attention(q, k, v, rpb):
    B, H, T, hd = q.shape
    rows = T // GRID_W
    kh = min(NA_ROWS_MAX, rows)
    kw = NA_COLS
    ncb = GRID_W // NA_QCOLS
    kg = k.reshape(B, H, rows, GRID_W, hd)
    vg = v.reshape(B, H, rows, GRID_W, hd)
    q_rows = jnp.moveaxis(q.reshape(B, H, rows, GRID_W, hd), 2, 0)

    r_idx = jnp.arange(rows)
    r_start = jnp.clip(r_idx - kh // 2, 0, rows - kh)
    c_idx = jnp.arange(GRID_W).reshape(ncb, NA_QCOLS)
    c_start = jnp.clip(c_idx - kw // 2, 0, GRID_W - kw)
    kc_start = jnp.clip(jnp.arange(ncb) * NA_QCOLS - kw // 2, 0, GRID_W - NA_KCOLS)
    kc = kc_start[:, None] + jnp.arange(NA_KCOLS)[None, :]
    col_valid = (kc[:, None, :] >= c_start[..., None]) & (kc[:, None, :] < c_start[..., None] + kw)
    valid = jnp.broadcast_to(col_valid[:, :, None, :], (ncb, NA_QCOLS, kh, NA_KCOLS))
    valid = valid.reshape(ncb, NA_QCOLS, kh * NA_KCOLS)
    dcol = jnp.clip(kc[:, None, :] - c_idx[..., None] + (kw - 1), 0, 2 * kw - 2)
    rpb_f = rpb.astype(jnp.float32)

    def one_row(args):
        q_r, r, rs = args
        k_r = lax.dynamic_slice_in_dim(kg, rs, kh, axis=2)
        v_r = lax.dynamic_slice_in_dim(vg, rs, kh, axis=2)

        def blocks(a):
            a = a[:, :, :, kc]
            return a.transpose(0, 1, 3, 2, 4, 5).reshape(B, H, ncb, kh * NA_KCOLS, hd)

        kb, vb = blocks(k_r), blocks(v_r)
        drow = rs + jnp.arange(kh) - r + (NA_ROWS_MAX - 1)
        bias = rpb_f[:, drow[None, None, :, None], dcol[:, :, None, :]]
        bias = bias.reshape(H, ncb, NA_QCOLS, kh * NA_KCOLS)
        qb = q_r.reshape(B, H, ncb, NA_QCOLS, hd)
        s = jnp.einsum('bhjqd,bhjkd->bhjqk', qb, kb, preferred_element_type=jnp.float32) * ATTN_SCALE
        s = jnp.where(valid, s + bias, NEG_INF)
        p = jax.nn.softmax(s, axis=-1)
        o = jnp.einsum('bhjqk,bhjkd->bhjqd', p.astype(v.dtype), vb, preferred_element_type=jnp.float32)
        return o.reshape(B, H, GRID_W, hd)

    o = lax.map(one_row, (q_rows, r_idx, r_start))
    return jnp.moveaxis(o, 0, 2).reshape(B, H, T, hd)


def _expert_choice_ffn(h, w_router, w_gate, w_up, w_down):
    B, T, D = h.shape
    cap = EC_CAPACITY * T // N_EXPERTS
    logits = jnp.einsum('btd,de->bte', h, w_router, preferred_element_type=jnp.float32)
    aff = jax.nn.softmax(logits, axis=-1)
    gate, idx = lax.top_k(aff.transpose(0, 2, 1), cap)
    bidx = jnp.arange(B)[:, None, None]
    xg = h[bidx, idx]

    def expert(args):
        xe, wg, wu, wd = args
        return (jax.nn.silu(xe @ wg) * (xe @ wu)) @ wd

    ye = lax.map(expert, (xg.transpose(1, 0, 2, 3), w_gate, w_up, w_down))
    ye = ye.transpose(1, 0, 2, 3) * gate[..., None].astype(h.dtype)
    return jnp.zeros_like(h).at[bidx, idx].add(ye)


def setup_inputs(seed: int = 0) -> dict:
    key = jax.random.key(seed)
    ks = jax.random.split(key, 13)
    f32 = jnp.float32
    nrm = lambda k, shape: jax.random.normal(k, shape, dtype=f32)
    return {
        "x": nrm(ks[0], (BATCH, SEQ, D_MODEL)),
        "attn_norm": 1.0 + 0.05 * nrm(ks[1], (DEPTH, D_MODEL)),
        "w_in": nrm(ks[2], (DEPTH, D_MODEL, 3 * D_MIX)) * D_MODEL ** -0.5,
        "dil_out_norm": 1.0 + 0.05 * nrm(ks[3], (DEPTH, D_DIL)),
        "na_out_norm": 1.0 + 0.05 * nrm(ks[4], (DEPTH, D_NA)),
        "na_rpb": 0.1 * nrm(ks[5], (DEPTH, N_HEADS_NA, 2 * NA_ROWS_MAX - 1, 2 * NA_COLS - 1)),
        "w_out": nrm(ks[6], (DEPTH, D_MIX, D_MODEL)) * D_MIX ** -0.5,
        "ffn_norm": 1.0 + 0.05 * nrm(ks[7], (DEPTH, D_MODEL)),
        "w_router": nrm(ks[8], (DEPTH, D_MODEL, N_EXPERTS)) * D_MODEL ** -0.5,
        "w_gate": nrm(ks[9], (DEPTH, N_EXPERTS, D_MODEL, EXPERT_FF)) * D_MODEL ** -0.5,
        "w_up": nrm(ks[10], (DEPTH, N_EXPERTS, D_MODEL, EXPERT_FF)) * D_MODEL ** -0.5,
        "w_down": nrm(ks[11], (DEPTH, N_EXPERTS, EXPERT_FF, D_MODEL)) * EXPERT_FF ** -0.5,
        "final_norm": 1.0 + 0.05 * nrm(ks[12], (D_MODEL,)),
    }


def reference(x, attn_norm, w_in, dil_out_norm, na_out_norm, na_rpb, w_out,
              ffn_norm, w_router, w_gate, w_up, w_down, final_norm):
    B, T, _ = x.shape
    cos, sin = _rope_tables(T)
    split_pts = (D_DIL, 2 * D_DIL, 3 * D_DIL, 3 * D_DIL + D_NA, 3 * D_DIL + 2 * D_NA)
    for l in range(DEPTH):
        h = _rmsnorm(x, attn_norm[l])
        proj = jnp.einsum('btd,df->btf', h, w_in[l])
        qa, ka, va, qb, kb, vb = jnp.split(proj, split_pts, axis=-1)
        qa = _apply_rope(_heads(qa, N_HEADS_DIL), cos, sin)
        ka = _apply_rope(_heads(ka, N_HEADS_DIL), cos, sin)
        oa = _dilated_attention(qa, ka, _heads(va, N_HEADS_DIL))
        ob = _neighbourhood_attention(_heads(qb, N_HEADS_NA), _heads(kb, N_HEADS_NA),
                                      _heads(vb, N_HEADS_NA), na_rpb[l])
        mixed = jnp.concatenate([_rmsnorm(_unheads(oa).astype(x.dtype), dil_out_norm[l]),
                                 _rmsnorm(_unheads(ob).astype(x.dtype), na_out_norm[l])], axis=-1)
        x = x + jnp.einsum('btf,fd->btd', mixed, w_out[l])
        x = x + _expert_choice_ffn(_rmsnorm(x, ffn_norm[l]), w_router[l], w_gate[l], w_up[l], w_down[l])
    return _rmsnorm(x, final_norm)
```

```python
import numpy as np
from contextlib import ExitStack
import concourse.bass as bass
import concourse.mybir as mybir
from concourse.bass_utils import run_bass_kernel_spmd

F32 = mybir.dt.float32
BF16 = mybir.dt.bfloat16
I32 = mybir.dt.int32
AF = mybir.ActivationFunctionType
ALU = mybir.AluOpType
AX = mybir.AxisListType

D = 1024
DEPTH = 2
NE = 16
FF = 2816
NF = FF // 128
EPS = 1e-6
NEG = -1e30
ENGS = ("sp", "act", "dve", "pe", "pool")


class _Op:
    __slots__ = ("eng", "fn", "reads", "writes", "dma", "deps", "src", "cnt", "sem", "tgt", "prev_tgt")

    def __init__(self, eng, fn, reads, writes, dma):
        self.eng = eng
        self.fn = fn
        self.reads = reads
        self.writes = writes
        self.dma = dma
        self.deps = []
        self.src = False
        self.cnt = 0
        self.sem = None
        self.tgt = 0
        self.prev_tgt = 0


class Sched:
    RING = {"sp": 8, "act": 4, "pool": 8}

    def __init__(self, nc, stack):
        self.nc = nc
        self.ops = []
        self.phase = 0
        self.sets = []
        for s in range(2):
            d = {}
            for e in ENGS:
                d[e] = stack.enter_context(nc.semaphore(f"pg{s}_{e}"))
            for e, n in self.RING.items():
                d["ring_" + e] = [stack.enter_context(nc.semaphore(f"dq{s}_{e}{i}")) for i in range(n)]
            self.sets.append(d)
        self.pring = [stack.enter_context(nc.semaphore(f"dqp_{i}")) for i in range(self.RING["pool"])]
        self.pring_cnt = [0] * self.RING["pool"]
        self.pring_pos = 0

    def op(self, eng, fn, reads=(), writes=(), dma=False):
        self.ops.append(_Op(eng, fn, tuple(reads), tuple(writes), dma))

    def flush(self):
        nc = self.nc
        ops = self.ops
        self.ops = []
        if not ops:
            return
        sset = self.sets[self.phase % 2]
        other = self.sets[(self.phase + 1) % 2]
        self.phase += 1
        last_w = {}
        readers = {}
        for i, o in enumerate(ops):
            deps = set()
            for k in o.reads:
                if k in last_w:
                    deps.add(last_w[k])
            for k in o.writes:
                if k in last_w:
                    deps.add(last_w[k])
                for r in readers.get(k, ()):
                    deps.add(r)
            deps.discard(i)
            for k in o.reads:
                readers.setdefault(k, []).append(i)
            for k in o.writes:
                last_w[k] = i
                readers[k] = []
            dl = []
            for d in sorted(deps):
                od = ops[d]
                if od.eng == "pe" and o.eng == "pe" and not od.dma and not o.dma:
                    continue
                dl.append(d)
                if not od.dma:
                    od.src = True
            o.deps = dl
        cnt = {e: 0 for e in ENGS}
        ring_pos = {e: 0 for e in self.RING}
        ring_cnt = {e: [0] * n for e, n in self.RING.items()}
        for o in ops:
            if o.dma and o.eng == "pool":
                r = self.pring_pos
                self.pring_pos = (r + 1) % len(self.pring)
                o.sem = self.pring[r]
                o.prev_tgt = self.pring_cnt[r]
                self.pring_cnt[r] += 16
                o.tgt = self.pring_cnt[r]
            elif o.dma:
                r = ring_pos[o.eng]
                ring_pos[o.eng] = (r + 1) % self.RING[o.eng]
                o.sem = sset["ring_" + o.eng][r]
                o.prev_tgt = ring_cnt[o.eng][r]
                ring_cnt[o.eng][r] += 16
                o.tgt = ring_cnt[o.eng][r]
            elif o.src:
                cnt[o.eng] += 1
                o.cnt = cnt[o.eng]
        by_eng = {e: [o for o in ops if o.eng == e] for e in ENGS}
        pring_final = list(self.pring_cnt)
        first = self.phase == 1

        def make_body(ename):
            def body(eng):
                waited = {}

                def wait(sem, val):
                    key = id(sem)
                    if waited.get(key, 0) >= val:
                        return
                    waited[key] = val
                    eng.wait_ge(sem, val)

                if ename == "pool" and not first:
                    for e in ENGS:
                        eng.sem_clear(other[e])
                    for e in self.RING:
                        if e == "pool":
                            continue
                        for s in other["ring_" + e]:
                            eng.sem_clear(s)
                for o in by_eng[ename]:
                    for d in o.deps:
                        od = ops[d]
                        if od.dma:
                            wait(od.sem, od.tgt)
                        else:
                            wait(sset[od.eng], od.cnt)
                    if o.dma and o.prev_tgt > 0:
                        wait(o.sem, o.prev_tgt)
                    ins = o.fn(eng)
                    if o.dma:
                        ins.then_inc(o.sem, 16)
                    elif o.src:
                        ins.then_inc(sset[ename], 1)
                if ename == "pool":
                    for r, s in enumerate(self.pring):
                        if pring_final[r] > 0:
                            wait(s, pring_final[r])
                elif ename in self.RING:
                    for r, s in enumerate(sset["ring_" + ename]):
                        if ring_cnt[ename][r] > 0:
                            wait(s, ring_cnt[ename][r])
            return body

        with nc.Block() as block:
            block.sync(make_body("sp"))
            block.scalar(make_body("act"))
            block.vector(make_body("dve"))
            block.tensor(make_body("pe"))
            block.gpsimd(make_body("pool"))


def _rope_table(T):
    pos = np.arange(T, dtype=np.float32)
    inv = (np.float32(10000.0) ** (-np.arange(0, 64, 2, dtype=np.float32) / np.float32(64))).astype(np.float32)
    ang = (pos[:, None] * inv[None, :]).astype(np.float32)
    c = np.cos(ang).astype(np.float32)
    s = np.sin(ang).astype(np.float32)
    c2 = np.concatenate([c, c], 1)
    s2 = np.concatenate([-s, s], 1)
    sc = np.float32(0.125)
    return np.ascontiguousarray(np.concatenate([c2 * sc, s2 * sc, c2, s2], 1).astype(np.float32))


def _dil_masks():
    q = np.arange(128)[:, None]
    kk = np.arange(256)[None, :]
    band = np.abs(kk - 64 - q) <= 64
    out = np.zeros((4, 128, 256), np.float32)
    for v in range(4):
        ok = band.copy()
        if v & 1:
            ok &= kk >= 64
        if v & 2:
            ok &= kk < 192
        out[v] = np.where(ok, 0.0, NEG)
    return out


def _na_var_blocks(nbn):
    return [0, 1, min(2, nbn - 1), nbn - 2, nbn - 1]


def _na_var(j, nbn):
    if j < 2:
        return j
    if j >= nbn - 2:
        return 3 + (j - (nbn - 2))
    return 2


def _na_bias(rpb, T):
    rows = T // 64
    kh = min(8, rows)
    nbn = T // 128
    L = rpb.shape[0]
    out = np.full((L, 5, 8, 128, 640), NEG, np.float32)
    qi = np.arange(128)
    qr = qi // 64
    qc = qi % 64
    kk = np.arange(640)
    for vi, j in enumerate(_na_var_blocks(nbn)):
        r = 2 * j + qr
        rs = np.clip(r - kh // 2, 0, rows - kh)
        cs = np.clip(qc - 8, 0, 64 - 16)
        krow = min(max(2 * j - 4, 0), rows - 10) + kk // 64
        kcol = kk % 64
        valid = ((krow[None, :] >= rs[:, None]) & (krow[None, :] < rs[:, None] + kh)
                 & (kcol[None, :] >= cs[:, None]) & (kcol[None, :] < cs[:, None] + 16))
        drow = np.clip(krow[None, :] - r[:, None] + 7, 0, 14)
        dcol = np.clip(kcol[None, :] - qc[:, None] + 15, 0, 30)
        g = rpb[:, :, drow, dcol]
        out[:, vi] = np.where(valid[None, None], g, np.float32(NEG))
    return out


def build(T, nph=None, dbg=False, ne_decl=NE):
    NT = T // 128
    CAP = 2 * T // NE
    NJ = CAP // 128
    CW = min(512, CAP)
    NCH = CAP // CW
    nc = bass.Bass("TRN2", target_bir_lowering=False)

    def din(name, shape):
        return nc.dram_tensor(name, shape, F32, kind="ExternalInput").ap()

    x_d = din("x", [T, D])
    attn_norm = din("attn_norm", [DEPTH, D])
    w_in = din("w_in", [DEPTH, D, 3 * D])
    dil_norm = din("dil_out_norm", [DEPTH, 512])
    na_norm = din("na_out_norm", [DEPTH, 512])
    w_out = din("w_out", [DEPTH, D, D])
    ffn_norm = din("ffn_norm", [DEPTH, D])
    w_router = din("w_router", [DEPTH, D, NE])
    w_gate = din("w_gate", [DEPTH, ne_decl, D, FF])
    w_up = din("w_up", [DEPTH, ne_decl, D, FF])
    w_down = din("w_down", [DEPTH, ne_decl, FF, D])
    final_norm = din("final_norm", [D])
    rope_d = din("rope_tab", [T, 256])
    dmask_d = din("dmask_tab", [4, 128, 256])
    nab_d = din("nab_tab", [DEPTH * 5 * 128, 8 * 640]).rearrange("(l v p) (h k) -> l v p h k", l=DEPTH, v=5, h=8)
    ident_d = din("ident_tab", [128, 128])
    tri_d = din("tri_tab", [128, 128])
    tokid_d = din("tokid_tab", [128, NT])
    out_d = nc.dram_tensor("out", [T, D], F32, kind="ExternalOutput").ap()

    def scr(name, shape, dt):
        return nc.dram_tensor(name, shape, dt, kind="ExternalOutput" if dbg else "Internal").ap()

    QKV = scr("s_qkv", [T, 3 * D], BF16)
    OD = [scr(f"s_od{p}", [T, 520], F32) for p in range(3)]
    ONA = scr("s_ona", [T, 512], F32)
    XS = [scr("s_xa", [T, D], F32), scr("s_xb", [T, D], F32)]
    H2 = scr("s_h2", [T, D], BF16)
    SLOT = [scr(f"s_slot{e}", [CAP, 2], F32) for e in range(NE)]

    _uid = [0]

    def uniq(n):
        _uid[0] += 1
        return f"{n}_{_uid[0]}"

    with ExitStack() as gst:
        S = Sched(nc, gst)

        def gsb(n, s, d):
            return gst.enter_context(nc.sbuf_tensor(n, s, d))

        identf = gsb("identf", [128, 128], F32)
        identb = gsb("identb", [128, 128], BF16)
        onesb = gsb("onesb", [128, 128], F32)
        trib = gsb("trib", [128, 128], F32)
        epst = gsb("epst", [128, 1], F32)
        AFF = gsb("AFF", [128, NT, NE], F32)
        tokid = gsb("tokid", [128, NT], F32)

        def DMA(q, out, in_, r=(), w=(), **kw):
            S.op(q, lambda e: e.dma_start(out=out, in_=in_, **kw), r, w, dma=True)

        def ACT(out, in_, func, r, w, **kw):
            S.op("act", lambda e: e.activation(out=out, in_=in_, func=func, **kw), r, w)

        def TT(out, in0, in1, op, r, w, eng="dve"):
            S.op(eng, lambda e: e.tensor_tensor(out=out, in0=in0, in1=in1, op=op), r, w)

        def TS(out, in0, s1, op0, r, w, s2=None, op1=None, eng="dve"):
            if op1 is None:
                S.op(eng, lambda e: e.tensor_scalar(out=out, in0=in0, scalar1=s1, scalar2=None, op0=op0), r, w)
            else:
                S.op(eng, lambda e: e.tensor_scalar(out=out, in0=in0, scalar1=s1, scalar2=s2, op0=op0, op1=op1), r, w)

        def STT(out, in0, scalar, in1, op0, op1, r, w, eng="dve"):
            S.op(eng, lambda e: e.scalar_tensor_tensor(out=out, in0=in0, scalar=scalar, in1=in1, op0=op0, op1=op1), r, w)

        def RED(out, in_, op, axis, r, w):
            S.op("dve", lambda e: e.tensor_reduce(out=out, in_=in_, axis=axis, op=op), r, w)

        def CP(out, in_, r, w, eng="dve"):
            S.op(eng, lambda e: e.tensor_copy(out=out, in_=in_), r, w)

        def RCP(out, in_, r, w):
            S.op("dve", lambda e: e.reciprocal(out=out, in_=in_), r, w)

        def MSET(ap, val, w, eng="dve"):
            S.op(eng, lambda e: e.memset(ap, val), (), w)

        def MM(out, lhsT, rhs, start, stop, r, w):
            S.op("pe", lambda e: e.matmul(out, lhsT=lhsT, rhs=rhs, start=start, stop=stop), r, w)

        def TR(out, in_, ident, r, w):
            S.op("pe", lambda e: e.transpose(out=out, in_=in_, identity=ident), r, w)

        def rms_rstd(ss, col_in, col_out, n, key):
            ACT(ss[:, col_out], ss[:, col_in], AF.Sqrt, [key], [key], bias=epst[:, 0:1], scale=1.0 / n)
            RCP(ss[:, col_out], ss[:, col_out], [key], [key])

        DMA("sp", identf[:], ident_d, w=["identf"])
        CP(identb[:], identf[:], ["identf"], ["identb"])
        MSET(onesb[:], 1.0, ["onesb"])
        MSET(epst[:], EPS, ["epst"])
        DMA("sp", trib[:], tri_d, w=["trib"])
        DMA("sp", tokid[:], tokid_d, w=["tokid"])
        S.flush()

        def phase_A(l, x_in):
            with ExitStack() as st:
                sb = lambda n, s, d: st.enter_context(nc.sbuf_tensor(uniq(n), s, d))
                pp = lambda n, s, d: st.enter_context(nc.psum_tensor(uniq(n), s, d))
                win = sb("a_win", [128, 8, 3 * D], BF16)
                gbc = sb("a_gbc", [128, D], F32)
                xt = [sb(f"a_xt{i}", [128, D], F32) for i in range(2)]
                junk = sb("a_junk", [128, D], F32)
                ss = [sb(f"a_ss{i}", [128, 2], F32) for i in range(2)]
                hb = [sb(f"a_hb{i}", [128, D], BF16) for i in range(2)]
                hT = [sb(f"a_hT{i}", [128, 8, 128], BF16) for i in range(2)]
                rp = [sb(f"a_rp{i}", [128, 256], F32) for i in range(2)]
                stg = [sb(f"a_stg{i}", [128, 3 * D], BF16) for i in range(2)]
                ta = sb("a_ta", [128, 8, 64], F32)
                tb = sb("a_tb", [128, 8, 64], F32)
                pTr = pp("a_pTr", [128, 8, 128], BF16)
                psA = [pp(f"a_ps{c}", [128, 512], F32) for c in range(6)]

                wv = w_in[l].rearrange("(k p) n -> p k n", p=128)
                for k in range(8):
                    DMA("pool", win[:, k, :], wv[:, k, :], w=[f"win{k}"], max_dma_last_dim=4096)
                DMA("sp", gbc[:], attn_norm[l].partition_broadcast(128), w=["gbc"])
                for i in range(NT):
                    b = i % 2
                    DMA("sp", xt[b][:], x_in[128 * i:128 * i + 128, :], w=[f"xt{b}"])
                    DMA("sp", rp[b][:], rope_d[128 * i:128 * i + 128, :], w=[f"rp{b}"])
                    ACT(junk[:], xt[b][:], AF.Square, [f"xt{b}"], ["junk", f"ss{b}"], accum_out=ss[b][:, 0:1])
                    rms_rstd(ss[b], slice(0, 1), slice(1, 2), D, f"ss{b}")
                    STT(hb[b][:], xt[b][:], ss[b][:, 1:2], gbc[:], ALU.mult, ALU.mult,
                        [f"xt{b}", f"ss{b}", "gbc"], [f"hb{b}"])
                    for k in range(8):
                        TR(pTr[:, k, :], hb[b][:, 128 * k:128 * k + 128], identb[:], [f"hb{b}", "identb"], ["pTr"])
                    ACT(hT[b][:], pTr[:], AF.Copy, ["pTr"], [f"hT{b}"])
                    for c in range(6):
                        for k in range(8):
                            MM(psA[c][:], hT[b][:, k, :], win[:, k, 512 * c:512 * c + 512], k == 0, k == 7,
                               [f"hT{b}", f"win{k}"], [f"psA{c}"])
                        dst = stg[b][:, 512 * c:512 * c + 512]
                        if c in (0, 1):
                            X = psA[c][:].rearrange("p (h e) -> p h e", e=64)
                            base = 0 if c == 0 else 128
                            Ct = rp[b][:, base:base + 64].unsqueeze(1).to_broadcast([128, 8, 64])
                            Sa = rp[b][:, base + 64:base + 96].unsqueeze(1).to_broadcast([128, 8, 32])
                            Sb = rp[b][:, base + 96:base + 128].unsqueeze(1).to_broadcast([128, 8, 32])
                            TT(ta[:], X, Ct, ALU.mult, [f"psA{c}", f"rp{b}"], ["ta"])
                            TT(tb[:, :, 0:32], X[:, :, 32:64], Sa, ALU.mult, [f"psA{c}", f"rp{b}"], ["tb"])
                            TT(tb[:, :, 32:64], X[:, :, 0:32], Sb, ALU.mult, [f"psA{c}", f"rp{b}"], ["tb"])
                            TT(dst.rearrange("p (h e) -> p h e", e=64), ta[:], tb[:], ALU.add, ["ta", "tb"], [f"stg{b}_{c}"])
                        elif c == 3:
                            ACT(dst, psA[c][:], AF.Copy, [f"psA{c}"], [f"stg{b}_{c}"], scale=0.125)
                        else:
                            ACT(dst, psA[c][:], AF.Copy, [f"psA{c}"], [f"stg{b}_{c}"])
                    DMA("pool", QKV[128 * i:128 * i + 128, :], stg[b][:], r=[f"stg{b}_{c}" for c in range(6)])
                S.flush()

        def phase_attn(l, kind):
            HB, W = (64, 256) if kind == "dil" else (256, 640)
            NBS = 8 if kind == "dil" else 4
            NKT = NBS + W // 128 - 1
            NCOL = 128 * NKT
            nW = W // 128
            with ExitStack() as st:
                sb = lambda n, s, d: st.enter_context(nc.sbuf_tensor(uniq(n), s, d))
                pp = lambda n, s, d: st.enter_context(nc.psum_tensor(uniq(n), s, d))
                KV = [sb(f"t_kv{i}", [128, NKT, 1024], BF16) for i in range(2)]
                KT = [sb(f"t_kt{i}", [128, 8, NCOL], BF16) for i in range(2)]
                QR = [sb(f"t_qr{i}", [128, NBS, 512], BF16) for i in range(2)]
                QT = [sb(f"t_qt{i}", [128, 4, 128 * NBS], BF16) for i in range(2)]
                P = [sb(f"t_p{i}", [128, 1024], BF16) for i in range(2)]
                PT = [sb(f"t_pt{i}", [128, 1024], BF16) for i in range(2)]
                ost = [sb(f"t_ost{i}", [128, 520], F32) for i in range(2)]
                mx = [sb(f"t_mx{i}", [128, 8], F32) for i in range(2)]
                nmx = [sb(f"t_nmx{i}", [128, 8], F32) for i in range(2)]
                den = [sb(f"t_den{i}", [128, 8], F32) for i in range(2)]
                rden = [sb(f"t_rden{i}", [128, 8], F32) for i in range(2)]
                lnd = [sb(f"t_lnd{i}", [128, 8], F32) for i in range(2)]
                if kind == "dil":
                    dm = sb("t_dm", [128, 4, 256], BF16)
                else:
                    nbi = sb("t_nbi", [128, 8, 640], BF16)
                    nbe = [sb(f"t_nbe{i}", [128, 640], BF16) for i in range(2)]
                    nbs_f = [sb(f"t_nbsf{i}", [128, 640], F32) for i in range(2)]
                pTr = pp("t_pTr", [128, 8, 128], BF16)
                Sp = [pp(f"t_S{i}", [128, 2, 512], F32) for i in range(2)]
                pPT = [pp(f"t_pPT{i}", [128, 1024], BF16) for i in range(2)]
                O = pp("t_O", [128, 512], F32)

                for i in range(2):
                    MSET(KV[i][:], 0.0, [f"KV{i}_{u}" for u in range(NKT)], eng="pool")
                    MSET(KT[i][:], 0.0, [f"KT{i}_{u}" for u in range(NKT)], eng="dve")
                if kind == "dil":
                    DMA("pool", dm[:], dmask_d.rearrange("v p k -> p v k"), w=["dm"])
                    qc0, kc0 = 0, 512
                else:
                    for h_ in range(8):
                        DMA("sp", nbs_f[h_ % 2][:], nab_d[l, 2, :, h_, :], w=[f"nbsf{h_ % 2}"])
                        CP(nbi[:, h_, :], nbs_f[h_ % 2][:], [f"nbsf{h_ % 2}"], ["nbi"])
                    qc0, kc0 = 1536, 2048

                cnt = {"seg": 0, "unit": 0, "blk": 0, "nbe": 0}

                def segment(pat, d, r, j0, nbs, L, nb):
                    sgi = cnt["seg"] % 2
                    cnt["seg"] += 1
                    qv = QKV.rearrange("(m dd) c -> dd m c", dd=d)
                    nkt = nbs + nW - 1
                    valid_u = []
                    for u in range(nkt):
                        pos0 = 128 * j0 - HB + 128 * u
                        a = max(0, -pos0)
                        bnd = min(128, L - pos0)
                        if bnd <= a:
                            continue
                        valid_u.append(u)
                        DMA("sp", KV[sgi][a:bnd, u, :], qv[r, pos0 + a:pos0 + bnd, kc0:kc0 + 1024], w=[f"KV{sgi}_{u}"])
                    for jj in range(nbs):
                        pos0 = 128 * (j0 + jj)
                        DMA("sp", QR[sgi][:, jj, :], qv[r, pos0:pos0 + 128, qc0:qc0 + 512], w=[f"QR{sgi}_{jj}"])
                    KT4 = KT[sgi][:].rearrange("p (pr ab) n -> p pr ab n", ab=2)
                    for u in valid_u:
                        for pr in range(4):
                            TR(pTr[:, pr, :], KV[sgi][:, u, 128 * pr:128 * pr + 128], identb[:], [f"KV{sgi}_{u}", "identb"], ["pTr"])
                        CP(KT4[0:64, :, 0, 128 * u:128 * u + 128], pTr[0:64, 0:4, :], ["pTr"], [f"KT{sgi}_{u}", "pTr"])
                        ACT(KT4[64:128, :, 1, 128 * u:128 * u + 128], pTr[64:128, 0:4, :], AF.Copy, ["pTr"], [f"KT{sgi}_{u}", "pTr"])
                    for jj in range(nbs):
                        for pr in range(4):
                            TR(pTr[:, 4 + pr, :], QR[sgi][:, jj, 128 * pr:128 * pr + 128], identb[:], [f"QR{sgi}_{jj}", "identb"], ["pTr"])
                        if jj % 2 == 0:
                            CP(QT[sgi][:, :, 128 * jj:128 * jj + 128], pTr[:, 4:8, :], ["pTr"], [f"QT{sgi}_{jj}", "pTr"])
                        else:
                            ACT(QT[sgi][:, :, 128 * jj:128 * jj + 128], pTr[:, 4:8, :], AF.Copy, ["pTr"], [f"QT{sgi}_{jj}", "pTr"])
                    for jj in range(nbs):
                        j = j0 + jj
                        bp = cnt["blk"] % 2
                        cnt["blk"] += 1
                        ktkeys = [f"KT{sgi}_{jj + c}" for c in range(nW)]
                        if kind == "dil":
                            var = (1 if j == 0 else 0) + (2 if j == nb - 1 else 0)
                            for hh in range(2):
                                up = cnt["unit"] % 2
                                cnt["unit"] += 1
                                Sv = Sp[up][:].rearrange("p b (h w) -> p (b h) w", w=256)
                                for hl in range(4):
                                    h = 4 * hh + hl
                                    MM(Sv[:, hl, :], identb[:], dm[:, var, :], True, False, ["identb", "dm"], [f"S{up}"])
                                    MM(Sv[:, hl, :], QT[sgi][:, h // 2, 128 * jj:128 * jj + 128],
                                       KT[sgi][:, h, 128 * jj:128 * jj + 256], False, True,
                                       [f"QT{sgi}_{jj}"] + ktkeys, [f"S{up}"])
                                RED(mx[bp][:, 4 * hh:4 * hh + 4], Sv, ALU.max, AX.X, [f"S{up}"], [f"mx{bp}"])
                                TS(nmx[bp][:, 4 * hh:4 * hh + 4], mx[bp][:, 4 * hh:4 * hh + 4], -1.0, ALU.mult, [f"mx{bp}"], [f"nmx{bp}"])
                                for hl in range(4):
                                    h = 4 * hh + hl
                                    ACT(P[up][:, 256 * hl:256 * hl + 256], Sv[:, hl, :], AF.Exp, [f"S{up}", f"nmx{bp}"],
                                        [f"P{up}", f"den{bp}_{h}"], bias=nmx[bp][:, h:h + 1], scale=1.0, accum_out=den[bp][:, h:h + 1])
                                for hl in range(4):
                                    for c in range(2):
                                        o0 = (2 * hl + c) * 128
                                        TR(pPT[up][:, o0:o0 + 128], P[up][:, 256 * hl + 128 * c:256 * hl + 128 * c + 128], identb[:],
                                           [f"P{up}", "identb"], [f"pPT{up}"])
                                if up == 0:
                                    CP(PT[up][:], pPT[up][:], [f"pPT{up}"], [f"PT{up}"])
                                else:
                                    ACT(PT[up][:], pPT[up][:], AF.Copy, [f"pPT{up}"], [f"PT{up}"])
                                for hl in range(4):
                                    h = 4 * hh + hl
                                    for c in range(2):
                                        o0 = (2 * hl + c) * 128
                                        MM(O[:, 64 * h:64 * h + 64], PT[up][:, o0:o0 + 128], KV[sgi][:, jj + c, 512 + 64 * h:512 + 64 * h + 64],
                                           c == 0, c == 1, [f"PT{up}", f"KV{sgi}_{jj + c}"], ["O"])
                        else:
                            var = _na_var(j, nb)
                            ws_ = min(max(2 * j - 4, 0), T // 64 - 10)
                            co = 64 * ws_ - (128 * j0 - HB)
                            ub = co // 128
                            ktkeys = [f"KT{sgi}_{ub + c}" for c in range(nW)]
                            for h in range(8):
                                up = cnt["unit"] % 2
                                cnt["unit"] += 1
                                if var == 2:
                                    bias_ap = nbi[:, h, :]
                                    bkey = "nbi"
                                else:
                                    eb = cnt["nbe"] % 2
                                    cnt["nbe"] += 1
                                    DMA("sp", nbs_f[eb][:], nab_d[l, var, :, h, :], w=[f"nbsf{eb}"])
                                    CP(nbe[eb][:], nbs_f[eb][:], [f"nbsf{eb}"], [f"nbe{eb}"])
                                    bias_ap = nbe[eb][:]
                                    bkey = f"nbe{eb}"
                                Sf = Sp[up][:].rearrange("p b w -> p (b w)")
                                for (c0, cw) in ((0, 512), (512, 128)):
                                    MM(Sf[:, c0:c0 + cw], identb[:], bias_ap[:, c0:c0 + cw], True, False, ["identb", bkey], [f"S{up}"])
                                    MM(Sf[:, c0:c0 + cw], QT[sgi][:, h // 2, 128 * jj:128 * jj + 128],
                                       KT[sgi][:, h, co + c0:co + c0 + cw], False, True,
                                       [f"QT{sgi}_{jj}"] + ktkeys, [f"S{up}"])
                                RED(mx[bp][:, h:h + 1], Sf[:, 0:640], ALU.max, AX.X, [f"S{up}"], [f"mx{bp}"])
                                TS(nmx[bp][:, h:h + 1], mx[bp][:, h:h + 1], -1.0, ALU.mult, [f"mx{bp}"], [f"nmx{bp}"])
                                ACT(P[up][:, 0:640], Sf[:, 0:640], AF.Exp,
                                    [f"S{up}", f"nmx{bp}"], [f"P{up}", f"den{bp}_{h}"], bias=nmx[bp][:, h:h + 1], scale=1.0,
                                    accum_out=den[bp][:, h:h + 1])
                                for c in range(5):
                                    TR(pPT[up][:, 128 * c:128 * c + 128], P[up][:, 128 * c:128 * c + 128], identb[:],
                                       [f"P{up}", "identb"], [f"pPT{up}"])
                                if up == 0:
                                    CP(PT[up][:, 0:640], pPT[up][:, 0:640], [f"pPT{up}"], [f"PT{up}"])
                                else:
                                    ACT(PT[up][:, 0:640], pPT[up][:, 0:640], AF.Copy, [f"pPT{up}"], [f"PT{up}"])
                                for c in range(5):
                                    MM(O[:, 64 * h:64 * h + 64], PT[up][:, 128 * c:128 * c + 128], KV[sgi][:, ub + c, 512 + 64 * h:512 + 64 * h + 64],
                                       c == 0, c == 4, [f"PT{up}", f"KV{sgi}_{ub + c}"], ["O"])
                        RCP(rden[bp][:], den[bp][:], [f"den{bp}_{h}" for h in range(8)], [f"rden{bp}"])
                        TT(ost[bp][:, 0:512].rearrange("p (h e) -> p h e", e=64), O[:].rearrange("p (h e) -> p h e", e=64),
                           rden[bp][:].unsqueeze(2).to_broadcast([128, 8, 64]), ALU.mult, ["O", f"rden{bp}"], [f"ost{bp}"])
                        pos0 = 128 * j
                        if kind == "dil":
                            ACT(lnd[bp][:], den[bp][:], AF.Ln, [f"den{bp}_{h}" for h in range(8)], [f"lnd{bp}"])
                            TT(ost[bp][:, 512:520], mx[bp][:], lnd[bp][:], ALU.add, [f"mx{bp}", f"lnd{bp}"], [f"ost{bp}"])
                            ov = OD[pat].rearrange("(m dd) c -> dd m c", dd=d)
                            DMA("pool", ov[r, pos0:pos0 + 128, :], ost[bp][:], r=[f"ost{bp}"])
                        else:
                            DMA("sp", ONA[pos0:pos0 + 128, :], ost[bp][:, 0:512], r=[f"ost{bp}"])

                if kind == "dil":
                    for pat, d in enumerate((1, 4, 16)):
                        L = T // d
                        nb = L // 128
                        for r in range(d):
                            for j0 in range(0, nb, NBS):
                                segment(pat, d, r, j0, min(NBS, nb - j0), L, nb)
                else:
                    nb = T // 128
                    for j0 in range(0, nb, NBS):
                        segment(0, 1, 0, j0, min(NBS, nb - j0), T, nb)
                S.flush()

        def phase_merge(l, x_in, x_out):
            with ExitStack() as st:
                sb = lambda n, s, d: st.enter_context(nc.sbuf_tensor(uniq(n), s, d))
                pp = lambda n, s, d: st.enter_context(nc.psum_tensor(uniq(n), s, d))
                wout = sb("m_wout", [128, 8, D], BF16)
                wr = sb("m_wr", [128, 8, NE], F32)
                gmix = sb("m_gmix", [128, D], F32)
                gffn = sb("m_gffn", [128, D], F32)
                od3 = [sb(f"m_od{i}", [128, 3, 520], F32) for i in range(2)]
                ona = [sb(f"m_ona{i}", [128, 512], F32) for i in range(2)]
                xt = [sb(f"m_xt{i}", [128, D], F32) for i in range(2)]
                mm_ = sb("m_m", [128, 8], F32)
                E = sb("m_E", [128, 3, 8], F32)
                sE = sb("m_sE", [128, 8], F32)
                acc = sb("m_acc", [128, 512], F32)
                t1 = sb("m_t1", [128, 512], F32)
                t2 = sb("m_t2", [128, 512], F32)
                junk = sb("m_junk", [128, D], F32)
                ss = [sb(f"m_ss{i}", [128, 8], F32) for i in range(2)]
                mixed = sb("m_mixed", [128, D], BF16)
                mixT = sb("m_mixT", [128, 8, 128], BF16)
                x1 = [sb(f"m_x1{i}", [128, D], F32) for i in range(2)]
                h2f = sb("m_h2f", [128, D], F32)
                h2b = [sb(f"m_h2b{i}", [128, D], BF16) for i in range(2)]
                h2T = sb("m_h2T", [128, 8, 128], F32)
                ex = sb("m_ex", [128, NE], F32)
                pTr = pp("m_pTr", [128, 8, 128], BF16)
                psO = [pp(f"m_psO{c}", [128, 512], F32) for c in range(2)]
                psR = pp("m_psR", [128, 8, 128], F32)
                psL = pp("m_psL", [128, NE], F32)

                wv = w_out[l].rearrange("(k p) n -> p k n", p=128)
                for k in range(8):
                    DMA("pool", wout[:, k, :], wv[:, k, :], w=[f"wout{k}"], max_dma_last_dim=4096)
                DMA("sp", wr[:], w_router[l].rearrange("(k p) n -> p k n", p=128), w=["wr"])
                DMA("sp", gmix[:, 0:512], dil_norm[l].partition_broadcast(128), w=["gmix"])
                DMA("sp", gmix[:, 512:1024], na_norm[l].partition_broadcast(128), w=["gmix"])
                DMA("sp", gffn[:], ffn_norm[l].partition_broadcast(128), w=["gffn"])
                for i in range(NT):
                    b = i % 2
                    rows = slice(128 * i, 128 * i + 128)
                    for p_ in range(3):
                        DMA("sp", od3[b][:, p_, :], OD[p_][rows, :], w=[f"od{b}"])
                    DMA("sp", ona[b][:], ONA[rows, :], w=[f"ona{b}"])
                    DMA("sp", xt[b][:], x_in[rows, :], w=[f"xt{b}"])
                    lse = od3[b][:, :, 512:520]
                    lse_t = lse.rearrange("q p h -> q h p")
                    RED(mm_[:], lse_t, ALU.max, AX.X, [f"od{b}"], ["mm"])
                    TT(E[:], lse, mm_[:].unsqueeze(1).to_broadcast([128, 3, 8]), ALU.subtract, [f"od{b}", "mm"], ["E"])
                    ACT(E[:], E[:], AF.Exp, ["E"], ["E"])
                    RED(sE[:], E[:].rearrange("q p h -> q h p"), ALU.add, AX.X, ["E"], ["sE"])
                    RCP(sE[:], sE[:], ["sE"], ["sE"])
                    TT(E[:], E[:], sE[:].unsqueeze(1).to_broadcast([128, 3, 8]), ALU.mult, ["E", "sE"], ["E"])

                    def wgt(p_):
                        return E[:, p_, :].unsqueeze(2).to_broadcast([128, 8, 64])

                    def ov(p_):
                        return od3[b][:, p_, 0:512].rearrange("q (h e) -> q h e", e=64)

                    a3 = acc[:].rearrange("q (h e) -> q h e", e=64)
                    TT(t1[:].rearrange("q (h e) -> q h e", e=64), ov(1), wgt(1), ALU.mult, [f"od{b}", "E"], ["t1"], eng="pool")
                    TT(t2[:].rearrange("q (h e) -> q h e", e=64), ov(2), wgt(2), ALU.mult, [f"od{b}", "E"], ["t2"], eng="pool")
                    TT(a3, ov(0), wgt(0), ALU.mult, [f"od{b}", "E"], ["acc"])
                    TT(acc[:], acc[:], t1[:], ALU.add, ["acc", "t1"], ["acc"])
                    TT(acc[:], acc[:], t2[:], ALU.add, ["acc", "t2"], ["acc"])
                    sk = f"ss{b}"
                    ACT(junk[:, 0:512], acc[:], AF.Square, ["acc"], ["junk", sk], accum_out=ss[b][:, 0:1])
                    ACT(junk[:, 512:1024], ona[b][:], AF.Square, [f"ona{b}"], ["junk", sk], accum_out=ss[b][:, 1:2])
                    rms_rstd(ss[b], slice(0, 2), slice(2, 4), 512, sk)
                    STT(mixed[:, 0:512], acc[:], ss[b][:, 2:3], gmix[:, 0:512], ALU.mult, ALU.mult, ["acc", sk, "gmix"], ["mixed"])
                    STT(mixed[:, 512:1024], ona[b][:], ss[b][:, 3:4], gmix[:, 512:1024], ALU.mult, ALU.mult, [f"ona{b}", sk, "gmix"], ["mixed"])
                    for k in range(8):
                        TR(pTr[:, k, :], mixed[:, 128 * k:128 * k + 128], identb[:], ["mixed", "identb"], ["pTr"])
                    ACT(mixT[:], pTr[:], AF.Copy, ["pTr"], ["mixT"])
                    for c in range(2):
                        for k in range(8):
                            MM(psO[c][:], mixT[:, k, :], wout[:, k, 512 * c:512 * c + 512], k == 0, k == 7, ["mixT", f"wout{k}"], [f"psO{c}"])
                        TT(x1[b][:, 512 * c:512 * c + 512], xt[b][:, 512 * c:512 * c + 512], psO[c][:], ALU.add, [f"xt{b}", f"psO{c}"], [f"x1{b}"])
                    DMA("pool", x_out[rows, :], x1[b][:], r=[f"x1{b}"])
                    ACT(junk[:], x1[b][:], AF.Square, [f"x1{b}"], ["junk", sk], accum_out=ss[b][:, 4:5])
                    rms_rstd(ss[b], slice(4, 5), slice(5, 6), D, sk)
                    STT(h2f[:], x1[b][:], ss[b][:, 5:6], gffn[:], ALU.mult, ALU.mult, [f"x1{b}", sk, "gffn"], ["h2f"])
                    ACT(h2b[b][:], h2f[:], AF.Copy, ["h2f"], [f"h2b{b}"])
                    DMA("pool", H2[rows, :], h2b[b][:], r=[f"h2b{b}"])
                    for k in range(8):
                        TR(psR[:, k, :], h2f[:, 128 * k:128 * k + 128], identf[:], ["h2f", "identf"], ["psR"])
                    CP(h2T[:], psR[:], ["psR"], ["h2T"])
                    for k in range(8):
                        MM(psL[:], h2T[:, k, :], wr[:, k, :], k == 0, k == 7, ["h2T", "wr"], ["psL"])
                    RED(ss[b][:, 6:7], psL[:], ALU.max, AX.X, ["psL"], [sk])
                    TS(ss[b][:, 6:7], ss[b][:, 6:7], -1.0, ALU.mult, [sk], [sk])
                    ACT(ex[:], psL[:], AF.Exp, ["psL", sk], ["ex", sk], bias=ss[b][:, 6:7], scale=1.0, accum_out=ss[b][:, 7:8])
                    RCP(ss[b][:, 7:8], ss[b][:, 7:8], [sk], [sk])
                    TS(AFF[:, i, :], ex[:], ss[b][:, 7:8], ALU.mult, ["ex", sk], ["AFF"])
                S.flush()

        def phase_topk():
            NIT = 28
            BIG = 1.0e6
            with ExitStack() as st:
                sb = lambda n, s, d: st.enter_context(nc.sbuf_tensor(uniq(n), s, d))
                pp = lambda n, s, d: st.enter_context(nc.psum_tensor(uniq(n), s, d))
                lo = sb("k_lo", [128, NE], F32)
                hi = sb("k_hi", [128, NE], F32)
                mid = sb("k_mid", [128, NE], F32)
                dd = sb("k_dd", [128, NE], F32)
                ge = sb("k_ge", [128, NE], F32)
                cmp_ = sb("k_cmp", [128, NT, NE], BF16)
                cntp = sb("k_cntp", [128, NE], F32)
                Mt = sb("k_Mt", [128, NE, NT], F32)
                rmul = sb("k_rmul", [128, NE, NT], F32)
                incl = sb("k_incl", [128, NE, NT], F32)
                base = sb("k_base", [128, NE], F32)
                sl = sb("k_sl", [128, NE, NT], F32)
                tt_ = sb("k_tt", [128, NE, NT], F32)
                sli = sb("k_sli", [128, NE, NT], I32)
                pay = sb("k_pay", [128, NE, NT, 2], F32)
                ptot = pp("k_ptot", [128, NE], F32)

                MSET(lo[:], 0.0, ["lo"])
                MSET(hi[:], 1.0, ["hi"])
                affT = AFF[:].rearrange("p t e -> p e t")
                for it in range(NIT):
                    TT(mid[:], lo[:], hi[:], ALU.add, ["lo", "hi"], ["mid"])
                    TS(mid[:], mid[:], 0.5, ALU.mult, ["mid"], ["mid"])
                    TT(cmp_[:], AFF[:], mid[:].unsqueeze(1).to_broadcast([128, NT, NE]), ALU.is_ge, ["AFF", "mid"], ["cmp"])
                    RED(cntp[:], cmp_[:].rearrange("p t e -> p e t"), ALU.add, AX.X, ["cmp"], ["cntp"])
                    MM(ptot[:], onesb[:], cntp[:], True, True, ["onesb", "cntp"], ["ptot"])
                    TS(ge[:], ptot[:], float(CAP), ALU.is_ge, ["ptot"], ["ge"])
                    TT(dd[:], mid[:], lo[:], ALU.subtract, ["mid", "lo"], ["dd"])
                    TT(dd[:], dd[:], ge[:], ALU.mult, ["dd", "ge"], ["dd"])
                    TT(lo[:], lo[:], dd[:], ALU.add, ["lo", "dd"], ["lo"])
                    TT(dd[:], hi[:], mid[:], ALU.subtract, ["hi", "mid"], ["dd"])
                    TT(dd[:], dd[:], ge[:], ALU.mult, ["dd", "ge"], ["dd"])
                    TT(hi[:], mid[:], dd[:], ALU.add, ["mid", "dd"], ["hi"])
                TT(Mt[:], affT, lo[:].unsqueeze(2).to_broadcast([128, NE, NT]), ALU.is_ge, ["AFF", "lo"], ["Mt"])
                MSET(rmul[:], 1.0, ["rmul"])
                MSET(rmul[:, :, 0:1], 0.0, ["rmul"])
                S.op("dve", lambda e: e.tensor_tensor_scan(out=incl[:].rearrange("p e t -> p (e t)"),
                                                           data0=rmul[:].rearrange("p e t -> p (e t)"),
                                                           data1=Mt[:].rearrange("p e t -> p (e t)"),
                                                           initial=0.0, op0=ALU.mult, op1=ALU.add),
                     ["rmul", "Mt"], ["incl"])
                CP(cntp[:], incl[:, :, NT - 1], ["incl"], ["cntp"])
                MM(ptot[:], trib[:], cntp[:], True, True, ["trib", "cntp"], ["ptot"])
                CP(base[:], ptot[:], ["ptot"], ["base"])
                TT(sl[:], incl[:], Mt[:], ALU.subtract, ["incl", "Mt"], ["sl"])
                TT(sl[:], sl[:], base[:].unsqueeze(2).to_broadcast([128, NE, NT]), ALU.add, ["sl", "base"], ["sl"])
                TS(tt_[:], Mt[:], -BIG, ALU.mult, ["Mt"], ["tt"], s2=BIG, op1=ALU.add)
                TT(sl[:], sl[:], tt_[:], ALU.add, ["sl", "tt"], ["sl"])
                CP(sli[:], sl[:], ["sl"], ["sli"])
                CP(pay[:, :, :, 0], tokid[:].unsqueeze(1).to_broadcast([128, NE, NT]), ["tokid"], ["pay"])
                CP(pay[:, :, :, 1], affT, ["AFF"], ["pay"])
                breg = {}

                for e_ in range(NE):
                    for t_ in range(NT):
                        def f(eng, e_=e_, t_=t_):
                            if "r" not in breg:
                                breg["r"] = eng.alloc_register(uniq("bc"))
                                eng.reg_mov(breg["r"], CAP - 1)
                            return eng.indirect_dma_start(
                                out=SLOT[e_], out_offset=bass.IndirectOffsetOnAxis(ap=sli[:, e_, t_:t_ + 1], axis=0),
                                in_=pay[:, e_, t_, :], in_offset=None, bounds_check=breg["r"], oob_is_err=False)
                        S.op("pool", f, ["sli", "pay"], [], dma=True)
                S.flush()

        def phase_ffn(l, x_acc):
            RG = 3
            with ExitStack() as st:
                sb = lambda n, s, d: st.enter_context(nc.sbuf_tensor(uniq(n), s, d))
                pp = lambda n, s, d: st.enter_context(nc.psum_tensor(uniq(n), s, d))
                SL = [sb(f"f_sl{i}", [128, NJ, 2], F32) for i in range(2)]
                idx = [sb(f"f_idx{i}", [128, NJ], I32) for i in range(2)]
                gate = [sb(f"f_gate{i}", [128, NJ], F32) for i in range(2)]
                Xg = sb("f_xg", [128, NJ, D], BF16)
                XgT = sb("f_xgT", [128, 8, CAP], BF16)
                AT = sb("f_AT", [128, NF, CAP], BF16)
                wdb = sb("f_wdb", [128, NF, D], BF16)
                sg_ = [sb(f"f_sg{i}", [128, 8, 128], F32) for i in range(RG)]
                su_ = [sb(f"f_su{i}", [128, 8, 128], F32) for i in range(RG)]
                sd_ = [sb(f"f_sd{i}", [128, D], F32) for i in range(RG)]
                wgb = [sb(f"f_wgb{i}", [128, 8, 128], BF16) for i in range(RG)]
                wub = [sb(f"f_wub{i}", [128, 8, 128], BF16) for i in range(RG)]
                sig = [sb(f"f_sig{i}", [128, CW], F32) for i in range(2)]
                ysc = [sb(f"f_ysc{i}", [128, D], F32) for i in range(2)]
                pTr = pp("f_pTr", [128, 8, 128], BF16)
                pG = [pp(f"f_pG{i}", [128, 512], F32) for i in range(2)]
                pU = [pp(f"f_pU{i}", [128, 512], F32) for i in range(2)]
                pY = [pp(f"f_pY{i}", [128, 512], F32) for i in range(2)]
                cnt = {"w": 0, "d": 0, "u": 0, "y": 0}

                def prefetch(e_):
                    b = e_ % 2
                    DMA("sp", SL[b][:], SLOT[e_].rearrange("(p j) f -> p j f", j=NJ), w=[f"SL{b}"])
                    CP(idx[b][:], SL[b][:, :, 0], [f"SL{b}"], [f"idx{b}"])
                    CP(gate[b][:], SL[b][:, :, 1], [f"SL{b}"], [f"gate{b}"])
                    for j in range(NJ):
                        def f(eng, j=j, b=b):
                            return eng.indirect_dma_start(out=Xg[:, j, :], out_offset=None, in_=H2,
                                                          in_offset=bass.IndirectOffsetOnAxis(ap=idx[b][:, j:j + 1], axis=0))
                        S.op("pool", f, [f"idx{b}"], [f"Xg{j}"], dma=True)

                def transposes(e_):
                    for j in range(NJ):
                        for k in range(8):
                            TR(pTr[:, k, :], Xg[:, j, 128 * k:128 * k + 128], identb[:], [f"Xg{j}", "identb"], ["pTr"])
                        if j % 2 == 0:
                            CP(XgT[:, :, 128 * j:128 * j + 128], pTr[:], ["pTr"], ["XgT"])
                        else:
                            ACT(XgT[:, :, 128 * j:128 * j + 128], pTr[:], AF.Copy, ["pTr"], ["XgT"])

                prefetch(0)
                transposes(0)
                for e_ in range(NE):
                    b = e_ % 2
                    wgv = w_gate[l, e_].rearrange("(k p) n -> p k n", p=128)
                    wuv = w_up[l, e_].rearrange("(k p) n -> p k n", p=128)
                    for f_ in range(NF):
                        wi = cnt["w"] % RG
                        cnt["w"] += 1
                        DMA("sp", sg_[wi][:], wgv[:, :, 128 * f_:128 * f_ + 128], w=[f"sg{wi}"])
                        DMA("sp", su_[wi][:], wuv[:, :, 128 * f_:128 * f_ + 128], w=[f"su{wi}"])
                        CP(wgb[wi][:], sg_[wi][:], [f"sg{wi}"], [f"wgb{wi}"], eng="pool")
                        ACT(wub[wi][:], su_[wi][:], AF.Copy, [f"su{wi}"], [f"wub{wi}"])
                        di = cnt["d"] % RG
                        cnt["d"] += 1
                        DMA("sp", sd_[di][:], w_down[l, e_, 128 * f_:128 * f_ + 128, :], w=[f"sd{di}"])
                        CP(wdb[:, f_, :], sd_[di][:], [f"sd{di}"], [f"wdb{f_}"])
                        for ch in range(NCH):
                            ui = cnt["u"] % 2
                            cnt["u"] += 1
                            cs = slice(CW * ch, CW * ch + CW)
                            for k in range(8):
                                MM(pG[ui][:, 0:CW], wgb[wi][:, k, :], XgT[:, k, cs], k == 0, k == 7, [f"wgb{wi}", "XgT"], [f"pG{ui}"])
                            for k in range(8):
                                MM(pU[ui][:, 0:CW], wub[wi][:, k, :], XgT[:, k, cs], k == 0, k == 7, [f"wub{wi}", "XgT"], [f"pU{ui}"])
                            ACT(sig[ui][:], pG[ui][:, 0:CW], AF.Silu, [f"pG{ui}"], [f"sig{ui}"])
                            TT(AT[:, f_, cs], sig[ui][:], pU[ui][:, 0:CW], ALU.mult, [f"sig{ui}", f"pU{ui}"], [f"AT{f_}"])
                    if e_ + 1 < NE:
                        prefetch(e_ + 1)
                    for j in range(NJ):
                        yb = cnt["y"] % 2
                        cnt["y"] += 1
                        for hd in range(2):
                            for f_ in range(NF):
                                MM(pY[hd][:], AT[:, f_, 128 * j:128 * j + 128], wdb[:, f_, 512 * hd:512 * hd + 512], f_ == 0, f_ == NF - 1,
                                   [f"AT{f_}", f"wdb{f_}"], [f"pY{hd}"])
                            ACT(ysc[yb][:, 512 * hd:512 * hd + 512], pY[hd][:], AF.Copy, [f"pY{hd}", f"gate{b}"], [f"ysc{yb}"],
                                scale=gate[b][:, j:j + 1])

                        def f(eng, j=j, b=b, yb=yb):
                            return eng.indirect_dma_start(out=x_acc, out_offset=bass.IndirectOffsetOnAxis(ap=idx[b][:, j:j + 1], axis=0),
                                                          in_=ysc[yb][:], in_offset=None, compute_op=ALU.add)
                        S.op("pool", f, [f"ysc{yb}", f"idx{b}", "xacc"], ["xacc"], dma=True)
                    if e_ + 1 < NE:
                        transposes(e_ + 1)
                S.flush()

        def phase_final(x_in):
            with ExitStack() as st:
                sb = lambda n, s, d: st.enter_context(nc.sbuf_tensor(uniq(n), s, d))
                gbc = sb("z_gbc", [128, D], F32)
                xt = [sb(f"z_xt{i}", [128, D], F32) for i in range(2)]
                yo = [sb(f"z_yo{i}", [128, D], F32) for i in range(2)]
                junk = sb("z_junk", [128, D], F32)
                ss = [sb(f"z_ss{i}", [128, 2], F32) for i in range(2)]
                DMA("sp", gbc[:], final_norm.partition_broadcast(128), w=["gbc"])
                for i in range(NT):
                    b = i % 2
                    rows = slice(128 * i, 128 * i + 128)
                    DMA("sp", xt[b][:], x_in[rows, :], w=[f"xt{b}"])
                    ACT(junk[:], xt[b][:], AF.Square, [f"xt{b}"], ["junk", f"ss{b}"], accum_out=ss[b][:, 0:1])
                    rms_rstd(ss[b], slice(0, 1), slice(1, 2), D, f"ss{b}")
                    STT(yo[b][:], xt[b][:], ss[b][:, 1:2], gbc[:], ALU.mult, ALU.mult, [f"xt{b}", f"ss{b}", "gbc"], [f"yo{b}"])
                    DMA("pool", out_d[rows, :], yo[b][:], r=[f"yo{b}"])
                S.flush()

        plan = []
        x_cur = x_d
        for l in range(DEPTH):
            x_nxt = XS[l % 2]
            plan.append(lambda l=l, x_cur=x_cur: phase_A(l, x_cur))
            plan.append(lambda l=l: phase_attn(l, "dil"))
            plan.append(lambda l=l: phase_attn(l, "na"))
            plan.append(lambda l=l, x_cur=x_cur, x_nxt=x_nxt: phase_merge(l, x_cur, x_nxt))
            plan.append(lambda: phase_topk())
            plan.append(lambda l=l, x_nxt=x_nxt: phase_ffn(l, x_nxt))
            x_cur = x_nxt
        plan.append(lambda x_cur=x_cur: phase_final(x_cur))
        for pi, ph in enumerate(plan):
            if nph is not None and pi >= nph:
                break
            ph()
    return nc


def _tables(T, na_rpb):
    NT = T // 128
    p = np.arange(128)
    return {
        "rope_tab": _rope_table(T),
        "dmask_tab": _dil_masks(),
        "nab_tab": np.ascontiguousarray(_na_bias(np.asarray(na_rpb, np.float32), T).transpose(0, 1, 3, 2, 4)).reshape(DEPTH * 5 * 128, 8 * 640),
        "ident_tab": np.eye(128, dtype=np.float32),
        "tri_tab": (p[:, None] < p[None, :]).astype(np.float32),
        "tokid_tab": (128.0 * np.arange(NT)[None, :] + p[:, None]).astype(np.float32),
    }


def make_in_maps(inputs, T, batch_ids):
    tabs = _tables(T, inputs["na_rpb"])
    shared = {k: np.ascontiguousarray(np.asarray(v, np.float32)) for k, v in inputs.items() if k not in ("x", "na_rpb")}
    shared.update(tabs)
    maps = []
    for b in batch_ids:
        m = dict(shared)
        m["x"] = np.ascontiguousarray(np.asarray(inputs["x"][b, :T], np.float32))
        maps.append(m)
    return maps


def kernel(**inputs):
    B, T, _ = inputs["x"].shape
    nc = build(T)
    G = 1
    outs = []
    for g0 in range(0, B, G):
        ids = list(range(g0, min(B, g0 + G)))
        maps = make_in_maps(inputs, T, ids)
        res = run_bass_kernel_spmd(nc, maps, core_ids=list(range(len(ids))))
        outs.extend(np.asarray(r["out"], np.float32) for r in res.results)
    return np.stack(outs, 0)
```
